# Optimizing a Trainium2 kernel written in Bass

```python
import math
import jax
import jax.numpy as jnp
from jax import lax
import numpy as np

D_MODEL = 1024
BATCH = 4
SEQ = 4096
DEPTH = 4

DN_HEADS = 8
DN_DK = 128
DN_DV = 128
DN_CHUNK = 64
CONV_WIDTH = 4
QK_DIM = DN_HEADS * DN_DK
V_DIM = DN_HEADS * DN_DV
N_QKV_DN = 2 * QK_DIM + V_DIM
MOBA_HEADS = 8
MOBA_HD = 64
MOBA_DIM = MOBA_HEADS * MOBA_HD
MOBA_BLOCK = 256
MOBA_TOPK = 3
MOBA_Q_CHUNK = 128
N_BUCKETS = 32
REL_MAX_DIST = 128
N_BRANCH = 2
N_IN = N_QKV_DN + V_DIM + 2 * DN_HEADS + 3 * MOBA_DIM + N_BRANCH * D_MODEL
D_FF = 2816
N_EXPERTS = 8
MOE_TOPK = 2
D_EXPERT = 3584
N_DENSE = (DEPTH + 1) // 2
N_MOE = DEPTH // 2
DEEPNORM_ALPHA = (2 * DEPTH) ** 0.25
DEEPNORM_BETA = (8 * DEPTH) ** -0.25
LN_EPS = 1e-5
RMS_EPS = 1e-6
F32 = jnp.float32

kernel_name = 'hybrid_deltanet_moba_moe_deepnorm'


def _layer_norm(x, g, b):
    xf = x.astype(F32)
    mu = jnp.mean(xf, axis=-1, keepdims=True)
    var = jnp.mean(jnp.square(xf - mu), axis=-1, keepdims=True)
    y = (xf - mu) * lax.rsqrt(var + LN_EPS) * g.astype(F32) + b.astype(F32)
    return y.astype(x.dtype)


def _l2norm(t):
    return t * lax.rsqrt(jnp.sum(jnp.square(t), axis=-1, keepdims=True) + RMS_EPS)


def _causal_conv(x, w):
    width, ch = w.shape
    return lax.conv_general_dilated(
        x, w[:, None, :], window_strides=(1,), padding=[(width - 1, 0)],
        dimension_numbers=('NWC', 'WIO', 'NWC'), feature_group_count=ch)


def _rel_bucket(dist):
    n = jnp.maximum(dist, 0)
    max_exact = N_BUCKETS // 2
    nf = jnp.maximum(n, 1).astype(F32)
    large = max_exact + (jnp.log(nf / max_exact) / math.log(REL_MAX_DIST / max_exact)
                         * (N_BUCKETS - max_exact)).astype(jnp.int32)
    large = jnp.minimum(large, N_BUCKETS - 1)
    return jnp.where(n < max_exact, n, large)


def _gated_delta_rule(q, k, v, g, beta):
    b_, s_, h_, dk = q.shape
    dv = v.shape[-1]
    nc = s_ // DN_CHUNK

    def to_chunks(t):
        t = jnp.moveaxis(t, 2, 1)
        return t.reshape((b_, h_, nc, DN_CHUNK) + t.shape[3:])

    q, k, v, g, beta = (to_chunks(t) for t in (q, k, v, g, beta))
    decay = jnp.cumsum(g, axis=-1)
    pos = jnp.arange(DN_CHUNK)
    lower = pos[:, None] >= pos[None, :]
    strict = pos[:, None] > pos[None, :]
    diff = decay[..., :, None] - decay[..., None, :]
    decay_mask = jnp.exp(jnp.where(lower, diff, -jnp.inf))
    k_beta = k * beta[..., None]
    v_beta = v * beta[..., None]
    kk = jnp.einsum('bhncd,bhnjd->bhncj', k_beta, k)
    a_mat = jnp.eye(DN_CHUNK, dtype=F32) + jnp.where(strict, kk * decay_mask, 0.0)
    rhs = jnp.concatenate([v_beta, k_beta * jnp.exp(decay)[..., None]], axis=-1)
    sol = lax.linalg.triangular_solve(a_mat, rhs, left_side=True, lower=True)
    value, k_cum = sol[..., :dv], sol[..., dv:]
    intra = jnp.einsum('bhncd,bhnjd->bhncj', q, k) * decay_mask
    q_dec = q * jnp.exp(decay)[..., None]
    k_dec = k * jnp.exp(decay[..., -1:] - decay)[..., None]
    g_last = jnp.exp(decay[..., -1])
    xs = tuple(jnp.moveaxis(t, 2, 0) for t in (q_dec, k_dec, value, k_cum, intra, g_last))

    def step(state, inp):
        qd, kd, val, kc, att, gl = inp
        v_new = val - jnp.einsum('bhck,bhkv->bhcv', kc, state)
        o = jnp.einsum('bhck,bhkv->bhcv', qd, state) + jnp.einsum('bhcj,bhjv->bhcv', att, v_new)
        state = state * gl[..., None, None] + jnp.einsum('bhck,bhcv->bhkv', kd, v_new)
        return state, o

    state0 = jnp.zeros((b_, h_, dk, dv), F32)
    _, o = lax.scan(step, state0, xs)
    o = jnp.moveaxis(o, 0, 2).reshape(b_, h_, s_, dv)
    return jnp.moveaxis(o, 1, 2)


def _moba_attention(q, k, v, rel_bias):
    b_, s_, h_, hd = q.shape
    s_pad = -(-s_ // MOBA_BLOCK) * MOBA_BLOCK
    pad = s_pad - s_
    q, k, v = (jnp.pad(jnp.moveaxis(t, 2, 1), ((0, 0), (0, 0), (0, pad), (0, 0))) for t in (q, k, v))
    q = q * (hd ** -0.5)
    nb = s_pad // MOBA_BLOCK
    k_blk = k.reshape(b_, h_, nb, MOBA_BLOCK, hd)
    v_blk = v.reshape(b_, h_, nb, MOBA_BLOCK, hd)
    k_mean = jnp.mean(k_blk.astype(F32), axis=3)
    own = jnp.arange(s_pad) // MOBA_BLOCK
    scores = jnp.einsum('bhsd,bhnd->bhsn', q.astype(F32), k_mean)
    fully_past = jnp.arange(nb)[None, :] < own[:, None]
    scores = jnp.where(fully_past, scores, -jnp.inf)
    n_sel = min(MOBA_TOPK, nb)
    _, sel = lax.top_k(scores, n_sel)
    sel_valid = jnp.arange(n_sel)[None, :] < own[:, None]

    nq = s_pad // MOBA_Q_CHUNK
    q_c = q.reshape(b_, h_, nq, MOBA_Q_CHUNK, hd).transpose(2, 0, 1, 3, 4)
    sel_c = sel.reshape(b_, h_, nq, MOBA_Q_CHUNK, n_sel).transpose(2, 0, 1, 3, 4)
    valid_c = sel_valid.reshape(nq, MOBA_Q_CHUNK, n_sel)
    start_c = jnp.arange(nq, dtype=jnp.int32) * MOBA_Q_CHUNK
    b_ix = jnp.arange(b_)[:, None, None, None]
    h_ix = jnp.arange(h_)[None, :, None, None]
    offs = jnp.arange(MOBA_BLOCK)

    def one_chunk(args):
        qc, sc, vc, start = args
        qpos = start + jnp.arange(MOBA_Q_CHUNK)
        ob = start // MOBA_BLOCK
        k_own = lax.dynamic_index_in_dim(k_blk, ob, axis=2, keepdims=False)
        v_own = lax.dynamic_index_in_dim(v_blk, ob, axis=2, keepdims=False)
        dist_own = qpos[:, None] - (ob * MOBA_BLOCK + offs)[None, :]
        l_own = jnp.einsum('bhqd,bhnd->bhqn', qc, k_own).astype(F32) + rel_bias[:, _rel_bucket(dist_own)]
        l_own = jnp.where(dist_own >= 0, l_own, -jnp.inf)
        k_sel = k_blk[b_ix, h_ix, sc]
        v_sel = v_blk[b_ix, h_ix, sc]
        dist_sel = qpos[:, None, None] - (sc[..., None] * MOBA_BLOCK + offs)
        l_sel = jnp.einsum('bhqd,bhqknd->bhqkn', qc, k_sel).astype(F32) + rel_bias[h_ix[..., None], _rel_bucket(dist_sel)]
        l_sel = jnp.where(vc[:, :, None], l_sel, -jnp.inf)
        logits = jnp.concatenate([l_own, l_sel.reshape(b_, h_, MOBA_Q_CHUNK, n_sel * MOBA_BLOCK)], axis=-1)
        p = jax.nn.softmax(logits, axis=-1).astype(v.dtype)
        p_own = p[..., :MOBA_BLOCK]
        p_sel = p[..., MOBA_BLOCK:].reshape(b_, h_, MOBA_Q_CHUNK, n_sel, MOBA_BLOCK)
        return (jnp.einsum('bhqn,bhnd->bhqd', p_own, v_own)
                + jnp.einsum('bhqkn,bhqknd->bhqd', p_sel, v_sel))

    o = lax.map(one_chunk, (q_c, sel_c, valid_c, start_c))
    o = o.transpose(1, 0, 3, 2, 4).reshape(b_, s_pad, h_, hd)
    return o[:, :s_]


def _swiglu(x, w_gate, w_up, w_down):
    h = jax.nn.silu(x @ w_gate) * (x @ w_up)
    return h @ w_down


def _moe_swiglu(x, router_w, router_b, w_gate, w_up, w_down):
    logits = (x @ router_w).astype(F32) + router_b.astype(F32)
    top_val, top_idx = lax.top_k(logits, MOE_TOPK)
    top_w = jax.nn.softmax(top_val, axis=-1)
    gates = jnp.sum(jax.nn.one_hot(top_idx, N_EXPERTS, dtype=F32) * top_w[..., None], axis=-2)
    gates = gates.astype(x.dtype)
    y = jnp.zeros_like(x)
    for e in range(N_EXPERTS):
        y = y + gates[..., e:e + 1] * _swiglu(x, w_gate[e], w_up[e], w_down[e])
    return y


def setup_inputs(seed: int = 0) -> dict:
    key = jax.random.key(seed)
    ks = jax.random.split(key, 24)

    def nrm(k, shape, scale):
        return jax.random.normal(k, shape, F32) * scale

    x = nrm(ks[0], (BATCH, SEQ, D_MODEL), 1.0)
    w_in = nrm(ks[1], (DEPTH, D_MODEL, N_IN), D_MODEL ** -0.5)
    conv_w = nrm(ks[2], (DEPTH, CONV_WIDTH, N_QKV_DN), CONV_WIDTH ** -0.5)
    a_log = jnp.log(jax.random.uniform(ks[3], (DEPTH, DN_HEADS), F32, 1.0, 16.0))
    dt = jnp.exp(jax.random.uniform(ks[4], (DEPTH, DN_HEADS), F32, math.log(1e-3), math.log(1e-1)))
    dt_bias = dt + jnp.log(-jnp.expm1(-dt))
    dn_norm_w = 1.0 + nrm(ks[5], (DEPTH, DN_DV), 0.02)
    w_up_a = nrm(ks[6], (DEPTH, V_DIM, D_MODEL), V_DIM ** -0.5)
    w_up_b = nrm(ks[7], (DEPTH, MOBA_DIM, D_MODEL), MOBA_DIM ** -0.5)
    w_o = nrm(ks[8], (DEPTH, D_MODEL, D_MODEL), DEEPNORM_BETA * D_MODEL ** -0.5)
    rel_bias = nrm(ks[9], (MOBA_HEADS, N_BUCKETS), 0.3)
    ln1_g = 1.0 + nrm(ks[10], (DEPTH, D_MODEL), 0.02)
    ln1_b = nrm(ks[11], (DEPTH, D_MODEL), 0.02)
    ln2_g = 1.0 + nrm(ks[12], (DEPTH, D_MODEL), 0.02)
    ln2_b = nrm(ks[13], (DEPTH, D_MODEL), 0.02)
    ffn_w_gate = nrm(ks[14], (N_DENSE, D_MODEL, D_FF), D_MODEL ** -0.5)
    ffn_w_up = nrm(ks[15], (N_DENSE, D_MODEL, D_FF), D_MODEL ** -0.5)
    ffn_w_down = nrm(ks[16], (N_DENSE, D_FF, D_MODEL), DEEPNORM_BETA * D_FF ** -0.5)
    router_w = nrm(ks[17], (N_MOE, D_MODEL, N_EXPERTS), D_MODEL ** -0.5)
    router_b = nrm(ks[18], (N_MOE, N_EXPERTS), 0.01)
    exp_w_gate = nrm(ks[19], (N_MOE, N_EXPERTS, D_MODEL, D_EXPERT), D_MODEL ** -0.5)
    exp_w_up = nrm(ks[20], (N_MOE, N_EXPERTS, D_MODEL, D_EXPERT), D_MODEL ** -0.5)
    exp_w_down = nrm(ks[21], (N_MOE, N_EXPERTS, D_EXPERT, D_MODEL), DEEPNORM_BETA * D_EXPERT ** -0.5)
    return {'x': x, 'w_in': w_in, 'conv_w': conv_w, 'a_log': a_log, 'dt_bias': dt_bias,
            'dn_norm_w': dn_norm_w, 'w_up_a': w_up_a, 'w_up_b': w_up_b, 'w_o': w_o,
            'rel_bias': rel_bias, 'ln1_g': ln1_g, 'ln1_b': ln1_b, 'ln2_g': ln2_g, 'ln2_b': ln2_b,
            'ffn_w_gate': ffn_w_gate, 'ffn_w_up': ffn_w_up, 'ffn_w_down': ffn_w_down,
            'router_w': router_w, 'router_b': router_b, 'exp_w_gate': exp_w_gate,
            'exp_w_up': exp_w_up, 'exp_w_down': exp_w_down}


def reference(x, w_in, conv_w, a_log, dt_bias, dn_norm_w, w_up_a, w_up_b, w_o, rel_bias,
              ln1_g, ln1_b, ln2_g, ln2_b, ffn_w_gate, ffn_w_up, ffn_w_down,
              router_w, router_b, exp_w_gate, exp_w_up, exp_w_down):
    b_, s_, _ = x.shape
    c0 = N_QKV_DN
    c1 = c0 + V_DIM
    c2 = c1 + DN_HEADS
    c3 = c2 + DN_HEADS
    c4 = c3 + 3 * MOBA_DIM
    for layer in range(DEPTH):
        proj = jnp.einsum('bsd,dn->bsn', x, w_in[layer])
        qkv_dn, z, a_in, b_in, qkv_mb, gate_in = jnp.split(proj, [c0, c1, c2, c3, c4], axis=-1)

        qkv_dn = jax.nn.silu(_causal_conv(qkv_dn, conv_w[layer].astype(x.dtype))).astype(F32)
        q_a, k_a, v_a = jnp.split(qkv_dn, [QK_DIM, 2 * QK_DIM], axis=-1)
        q_a = _l2norm(q_a.reshape(b_, s_, DN_HEADS, DN_DK)) * (DN_DK ** -0.5)
        k_a = _l2norm(k_a.reshape(b_, s_, DN_HEADS, DN_DK))
        v_a = v_a.reshape(b_, s_, DN_HEADS, DN_DV)
        beta = jax.nn.sigmoid(b_in.astype(F32))
        g = -jnp.exp(a_log[layer].astype(F32)) * jax.nn.softplus(a_in.astype(F32) + dt_bias[layer].astype(F32))
        o_a = _gated_delta_rule(q_a, k_a, v_a, g, beta)
        o_a = (o_a * lax.rsqrt(jnp.mean(jnp.square(o_a), axis=-1, keepdims=True) + RMS_EPS)
               * dn_norm_w[layer].astype(F32)
               * jax.nn.silu(z.astype(F32).reshape(b_, s_, DN_HEADS, DN_DV)))
        y_a = jnp.einsum('bse,ed->bsd', o_a.reshape(b_, s_, V_DIM).astype(x.dtype), w_up_a[layer])

        q_b, k_b, v_b = (t.reshape(b_, s_, MOBA_HEADS, MOBA_HD) for t in jnp.split(qkv_mb, 3, axis=-1))
        o_b = _moba_attention(q_b, k_b, v_b, rel_bias)
        y_b = jnp.einsum('bse,ed->bsd', o_b.reshape(b_, s_, MOBA_DIM), w_up_b[layer])

        g_a, g_b = jnp.split(jax.nn.sigmoid(gate_in), N_BRANCH, axis=-1)
        mix = jnp.einsum('bsd,de->bse', g_a * y_a + g_b * y_b, w_o[layer])
        x = _layer_norm(DEEPNORM_ALPHA * x + mix, ln1_g[layer], ln1_b[layer])

        i = layer // 2
        if layer % 2 == 0:
            ffn = _swiglu(x, ffn_w_gate[i], ffn_w_up[i], ffn_w_down[i])
        else:
            ffn = _moe_swiglu(x, router_w[i], router_b[i], exp_w_gate[i], exp_w_up[i], exp_w_down[i])
        x = _layer_norm(DEEPNORM_ALPHA * x + ffn, ln2_g[layer], ln2_b[layer])
    return x
```

```python
import contextlib
import numpy as np
import concourse.bass as bass
import concourse.mybir as mybir
from concourse.bass_utils import run_bass_kernel_spmd

F32 = mybir.dt.float32
BF16 = mybir.dt.bfloat16
AF = mybir.ActivationFunctionType
ALU = mybir.AluOpType
AX = mybir.AxisListType

ENGS = ("pe", "act", "dve", "pool", "sp")
NRING = 8


class Buf:
    __slots__ = ("name", "w", "rd")

    def __init__(self, name=""):
        self.name = name
        self.w = None
        self.rd = []


class Op:
    __slots__ = ("eng", "fn", "waits", "sig", "dma", "sem", "val", "ring_wait")

    def __init__(self, eng, fn, dma):
        self.eng = eng
        self.fn = fn
        self.dma = dma
        self.waits = []
        self.sig = dma
        self.sem = None
        self.val = 0
        self.ring_wait = None


class Prog:
    def __init__(self, nc):
        self.nc = nc
        self.ops = {e: [] for e in ENGS}
        self.stack = contextlib.ExitStack()
        self.n = 0

    def sb(self, name, shape, dt):
        return self.stack.enter_context(self.nc.sbuf_tensor(name, list(shape), dt))

    def ps(self, name, shape, dt=F32):
        return self.stack.enter_context(self.nc.psum_tensor(name, list(shape), dt))

    def dram(self, name, shape, dt, kind="Internal"):
        if kind == "Internal":
            return self.nc.dram_tensor(name, list(shape), dt).ap()
        return self.nc.dram_tensor(name, list(shape), dt, kind=kind).ap()

    def op(self, eng, fn, rd=(), wr=(), dma=False):
        o = Op(eng, fn, dma)
        deps = {}
        for b in rd:
            if b.w is not None:
                deps[id(b.w)] = (b.w, True)
        for b in wr:
            if b.w is not None:
                deps[id(b.w)] = (b.w, True)
            for r in b.rd:
                if id(r) not in deps:
                    deps[id(r)] = (r, False)
        for d, hard in deps.values():
            if d is o:
                continue
            if d.dma or o.dma or d.eng != eng:
                need = True
            elif eng == "pe":
                need = False
            else:
                need = hard
            if need:
                d.sig = True
                o.waits.append(d)
        for b in rd:
            b.rd.append(o)
        for b in wr:
            b.w = o
            b.rd = []
        self.ops[eng].append(o)
        self.n += 1
        return o

    def dma(self, eng, out, in_, rd=(), wr=()):
        return self.op(eng, lambda e: e.dma_start(out=out, in_=in_), rd=rd, wr=wr, dma=True)

    def fence(self, eng, bufs):
        return self.op(eng, None, rd=bufs, wr=())

    def emit(self):
        nc = self.nc
        st = self.stack
        esem = {e: st.enter_context(nc.semaphore("s_" + e)) for e in ENGS}
        rings = {e: [st.enter_context(nc.semaphore("r_%s%d" % (e, i))) for i in range(NRING)]
                 for e in ("sp", "pool", "act")}
        for e in ENGS:
            cnt = 0
            nd = 0
            for o in self.ops[e]:
                if o.dma:
                    r = rings[e][nd % NRING]
                    o.sem = r
                    o.val = 16 * (nd // NRING + 1)
                    if nd >= NRING:
                        o.ring_wait = (r, 16 * (nd // NRING))
                    nd += 1
                elif o.sig:
                    cnt += 1
                    o.sem = esem[e]
                    o.val = cnt
        block = st.enter_context(nc.Block())

        def run(e, eng):
            waited = {}
            for o in self.ops[e]:
                ws = [(d.sem, d.val) for d in o.waits]
                if o.ring_wait is not None:
                    ws.append(o.ring_wait)
                for sem, val in ws:
                    k = id(sem)
                    if waited.get(k, 0) < val:
                        eng.wait_ge(sem, val)
                        waited[k] = val
                if o.fn is None:
                    continue
                ins = o.fn(eng)
                if o.dma:
                    ins.then_inc(o.sem, 16)
                elif o.sig:
                    ins.then_inc(o.sem, 1)

        @block.tensor
        def _(eng):
            run("pe", eng)

        @block.scalar
        def _(eng):
            run("act", eng)

        @block.vector
        def _(eng):
            run("dve", eng)

        @block.gpsimd
        def _(eng):
            run("pool", eng)

        @block.sync
        def _(eng):
            run("sp", eng)

    def close(self):
        self.stack.close()


def bcast_mid(ap2d, n):
    p, f = ap2d.shape
    return ap2d.unsqueeze(1).to_broadcast([p, n, f])


def bcast_last(ap2d, n):
    p, h = ap2d.shape
    return ap2d.unsqueeze(2).to_broadcast([p, h, n])


LN_EPS = 1e-5


def rsqrt_eps(P, out, in_, eps, RD, WR):
    P.op("dve", lambda e: e.tensor_scalar(out=out, in0=in_, scalar1=float(eps), scalar2=None, op0=ALU.add), rd=RD, wr=WR)
    P.op("dve", lambda e: e.reciprocal(out=out, in_=out), rd=WR, wr=WR)
    P.op("act", lambda e: e.activation(out=out, in_=out, func=AF.Sqrt), rd=WR, wr=WR)


def layer_norm(P, r32, R32, scr, SCR, gcol, bcol, out32, OUT32, ones_m, psA, PSA, psB, PSB, tmp, TMP, NC8=8, W=512, extra=()):
    SCRS = list(SCR) if isinstance(SCR, (list, tuple)) else [SCR]
    OUTS = list(OUT32) if isinstance(OUT32, (list, tuple)) else [OUT32]
    extra = list(extra)
    P.op("act", lambda e: e.activation(out=scr, in_=r32, func=AF.Square), rd=[R32], wr=SCRS)
    for c in range(NC8):
        P.op("pe", lambda e, c=c: e.matmul(psA, ones_m, r32[:, c, :], start=(c == 0), stop=(c == NC8 - 1)),
             rd=[R32] + extra, wr=[PSA])
    for c in range(NC8):
        P.op("pe", lambda e, c=c: e.matmul(psB, ones_m, scr[:, c, :], start=(c == 0), stop=(c == NC8 - 1)),
             rd=SCRS + extra, wr=[PSB])
    mean = tmp[:, 0, :]
    m2 = tmp[:, 1, :]
    rstd = tmp[:, 2, :]
    P.op("act", lambda e: e.activation(out=mean, in_=psA, func=AF.Copy), rd=[PSA], wr=[TMP])
    P.op("act", lambda e: e.activation(out=m2, in_=psA, func=AF.Square), rd=[PSA], wr=[TMP])
    P.op("dve", lambda e: e.tensor_tensor(out=rstd, in0=psB, in1=m2, op=ALU.subtract), rd=[PSB, TMP], wr=[TMP])
    rsqrt_eps(P, rstd, rstd, LN_EPS, [TMP], [TMP])
    P.op("dve", lambda e: e.tensor_tensor(out=scr, in0=r32, in1=bcast_mid(mean, NC8), op=ALU.subtract),
         rd=[R32, TMP], wr=SCRS)
    P.op("dve", lambda e: e.tensor_tensor(out=scr, in0=scr, in1=bcast_mid(rstd, NC8), op=ALU.mult),
         rd=SCRS + [TMP], wr=SCRS)
    for c in range(NC8):
        P.op("act", lambda e, c=c: e.activation(out=out32[:, c, :], in_=scr[:, c, :], func=AF.Identity,
                                                  bias=bcol[:, c:c + 1], scale=gcol[:, c:c + 1]),
             rd=SCRS + extra, wr=OUTS)


def build_p4b(Tc, F, E, alpha, FG=4, debug=0):
    nc = bass.Bass("TRN2", target_bir_lowering=False)
    P = Prog(nc)
    Em = max(E, 1)
    NT = Tc // 512
    NF = F // 128
    NG = (NF + FG - 1) // FG
    x1T = P.dram("x1T", [1024, Tc], F32, "ExternalInput")
    lnp = P.dram("lnp", [128, 2, 8], F32, "ExternalInput")
    wg = P.dram("wg", [Em, 1024, F], F32, "ExternalInput")
    wu = P.dram("wu", [Em, 1024, F], F32, "ExternalInput")
    wd = P.dram("wd", [Em, F, 1024], F32, "ExternalInput")
    if E:
        rw = P.dram("rw", [1024, E], F32, "ExternalInput")
        rb = P.dram("rb", [1, E], F32, "ExternalInput")
        ident_d = P.dram("ident", [128, 128], F32, "ExternalInput")
    x2T = P.dram("x2T", [1024, Tc], F32, "ExternalOutput")
    x1v = x1T.rearrange("(c p) t -> p c t", p=128)
    x2v = x2T.rearrange("(c p) t -> p c t", p=128)

    x1b = P.sb("x1b", [128, 8, Tc], BF16)
    X1B = Buf("x1b")
    acc = P.sb("acc", [128, 8, Tc], F32)
    ACC = [Buf("acc%d" % t) for t in range(NT)]
    acc2 = P.sb("acc2", [128, 8, Tc], F32) if debug in (9, 10, 11) else None
    lnp_s = P.sb("lnp_s", [128, 2, 8], F32)
    LNP = Buf()
    ones_m = P.sb("ones_m", [128, 128], F32)
    ONES = Buf()
    wbuf = P.sb("wbuf", [128, 2, 3 * 4096], BF16)
    assert FG == 4
    wgs = [wbuf[:, i, 0:4096].rearrange("p (c f) -> p c f", c=8) for i in range(2)]
    wus = [wbuf[:, i, 4096:8192].rearrange("p (c f) -> p c f", c=8) for i in range(2)]
    wds = [wbuf[:, i, 8192:12288].rearrange("p (j d) -> p j d", j=FG) for i in range(2)]
    WG = [Buf(), Buf()]
    WU = [Buf(), Buf()]
    WD = [Buf(), Buf()]
    ovl = [wbuf[:, i, :].bitcast(F32)[:, 0:4096].rearrange("p (c t) -> p c t", c=8) for i in range(2)]
    hs = [P.sb("hs%d" % i, [128, FG, 512], BF16) for i in range(2)]
    HS = [Buf(), Buf()]
    sg = [P.sb("sg%d" % i, [128, 512], F32) for i in range(2)]
    SG = [Buf(), Buf()]
    psG = [P.ps("psG%d" % i, [128, 512]) for i in range(2)]
    psU = [P.ps("psU%d" % i, [128, 512]) for i in range(2)]
    psD = [P.ps("psD%d" % i, [128, 512]) for i in range(2)]
    PSG = [Buf(), Buf()]
    PSU = [Buf(), Buf()]
    PSD = [Buf(), Buf()]
    psX = P.ps("psX", [128, 512])
    PSX = Buf()
    psY = P.ps("psY", [128, 512])
    PSY = Buf()

    P.dma("pool", x1b[:, :, :], x1v, wr=[X1B])
    P.dma("sp", lnp_s[:, :, :], lnp, wr=[LNP])
    P.op("dve", lambda e: e.memset(ones_m[:, :], 1.0 / 1024.0), wr=[ONES])

    gateB = None
    if E:
        NTK = Tc // 128
        rws = P.sb("rws", [128, 8, E], F32)
        RWS = Buf()
        rbs = P.sb("rbs", [128, E], F32)
        RBS = Buf()
        ident = P.sb("ident_s", [128, 128], F32)
        IDN = Buf()
        P.dma("sp", rws[:, :, :], rw.rearrange("(c p) e -> p c e", p=128), wr=[RWS])
        P.dma("sp", rbs[:, :], rb.partition_broadcast(128), wr=[RBS])
        P.dma("sp", ident[:, :], ident_d, wr=[IDN])
        lg = P.sb("lg", [128, NTK, E], F32)
        LG = Buf()
        gt = P.sb("gt", [128, NTK, E], F32)
        GT = Buf()
        m1 = P.sb("m1", [128, NTK], F32)
        M1 = Buf()
        m2 = P.sb("m2r", [128, NTK], F32)
        M2 = Buf()
        for t in range(NT):
            P.dma("sp", ovl[1], x1v[:, :, t * 512:(t + 1) * 512], wr=[WG[1], WU[1], WD[1]])
            for s in range(4):
                tk = t * 4 + s
                for c in range(8):
                    P.op("pe", lambda e, t=t, s=s, c=c, tk=tk: e.matmul(
                        psX[:, tk * E:(tk + 1) * E], ovl[1][:, c, s * 128:(s + 1) * 128], rws[:, c, :],
                        start=(c == 0), stop=(c == 7)), rd=[WG[1], RWS], wr=[PSX])
        psXv = psX[:, 0:NTK * E].rearrange("p (k e) -> p k e", e=E)
        P.op("dve", lambda e: e.tensor_tensor(out=lg[:, :, :], in0=psXv, in1=bcast_mid(rbs[:, :], NTK), op=ALU.add),
             rd=[PSX, RBS], wr=[LG])
        P.op("dve", lambda e: e.tensor_reduce(out=m1[:, :], in_=lg[:, :, :], axis=AX.X, op=ALU.max), rd=[LG], wr=[M1])
        P.op("dve", lambda e: e.tensor_tensor(out=gt[:, :, :], in0=lg[:, :, :], in1=bcast_last(m1[:, :], E), op=ALU.is_ge),
             rd=[LG, M1], wr=[GT])
        P.op("dve", lambda e: e.scalar_tensor_tensor(out=gt[:, :, :], in0=gt[:, :, :], scalar=-1e30, in1=lg[:, :, :],
                                                      op0=ALU.mult, op1=ALU.add), rd=[GT, LG], wr=[GT])
        P.op("dve", lambda e: e.tensor_reduce(out=m2[:, :], in_=gt[:, :, :], axis=AX.X, op=ALU.max), rd=[GT], wr=[M2])
        P.op("dve", lambda e: e.tensor_tensor(out=gt[:, :, :], in0=lg[:, :, :], in1=bcast_last(m2[:, :], E), op=ALU.is_ge),
             rd=[LG, M2], wr=[GT])
        P.op("dve", lambda e: e.tensor_tensor(out=lg[:, :, :], in0=lg[:, :, :], in1=bcast_last(m1[:, :], E), op=ALU.subtract),
             rd=[LG, M1], wr=[LG])
        P.op("act", lambda e: e.activation(out=lg[:, :, :], in_=lg[:, :, :], func=AF.Exp), rd=[LG], wr=[LG])
        P.op("dve", lambda e: e.tensor_tensor(out=gt[:, :, :], in0=gt[:, :, :], in1=lg[:, :, :], op=ALU.mult),
             rd=[GT, LG], wr=[GT])
        P.op("dve", lambda e: e.tensor_reduce(out=m1[:, :], in_=gt[:, :, :], axis=AX.X, op=ALU.add), rd=[GT], wr=[M1])
        P.op("dve", lambda e: e.reciprocal(out=m1[:, :], in_=m1[:, :]), rd=[M1], wr=[M1])
        P.op("dve", lambda e: e.tensor_tensor(out=gt[:, :, :], in0=gt[:, :, :], in1=bcast_last(m1[:, :], E), op=ALU.mult),
             rd=[GT, M1], wr=[GT])
        gT = P.sb("gT", [E, Tc], F32)
        GTT = Buf()
        for t in range(NT):
            for s in range(4):
                tk = t * 4 + s
                P.op("pe", lambda e, tk=tk, s=s: e.transpose(psY[0:E, s * 128:(s + 1) * 128], gt[:, tk, :], ident[:, :]),
                     rd=[GT, IDN], wr=[PSY])
            P.op("act", lambda e, t=t: e.activation(out=gT[:, t * 512:(t + 1) * 512], in_=psY[0:E, :], func=AF.Copy),
                 rd=[PSY], wr=[GTT])
        selm = P.sb("selm", [E, E, 128], F32)
        SELM = Buf()
        P.op("dve", lambda e: e.tensor_copy(out=selm[:, :, :], in_=bcast_last(ident[0:E, 0:E], 128)), rd=[IDN], wr=[SELM])
        gateB = [P.sb("gateB0", [128, Tc], F32)] * 2
        GB = [Buf()] * 2

    def gsz(g):
        return min(FG, NF - g * FG)

    def load_group(ex, g, slot):
        n = gsz(g)
        fs = slice(g * FG * 128, (g * FG + n) * 128)
        P.dma("pool", wgs[slot][:, :, 0:n * 128], wg[ex].rearrange("(c p) f -> p c f", p=128)[:, :, fs], wr=[WG[slot]])
        P.dma("pool", wus[slot][:, :, 0:n * 128], wu[ex].rearrange("(c p) f -> p c f", p=128)[:, :, fs], wr=[WU[slot]])
        P.dma("pool", wds[slot][:, 0:n, :], wd[ex][fs, :].rearrange("(j p) d -> p j d", p=128), wr=[WD[slot]])

    groups = [(ex, g) for ex in range(Em) for g in range(NG)]
    if debug == 7:
        groups = groups[::-1]
    SLOT0 = 1 if debug == 5 else 0
    load_group(groups[0][0], groups[0][1], SLOT0)
    it = 0
    for gi, (ex, g) in enumerate(groups):
        slot = (gi + SLOT0) % 2
        if gi + 1 < len(groups):
            load_group(groups[gi + 1][0], groups[gi + 1][1], (gi + 1 + SLOT0) % 2)
        if E and g == 0:
            gb = gateB[ex % 2]
            for t in range(NT):
                P.op("pe", lambda e, t=t, ex=ex: e.matmul(psY[:, :], selm[:, ex, :], gT[:, t * 512:(t + 1) * 512],
                                                          start=True, stop=True), rd=[SELM, GTT], wr=[PSY])
                P.op("act", lambda e, t=t, gb=gb: e.activation(out=gb[:, t * 512:(t + 1) * 512], in_=psY[:, :], func=AF.Copy),
                     rd=[PSY], wr=[GB[ex % 2]])
        ng = gsz(g)
        for t in range(NT):
            ts_ = slice(t * 512, (t + 1) * 512)
            hb = hs[it % 2]
            HB = HS[it % 2]
            for j in range(ng):
                q = (it * FG + j) % 2
                for c in range(8):
                    P.op("pe", lambda e, c=c, j=j, q=q, ts_=ts_, slot=slot: e.matmul(
                        psG[q][:, :], wgs[slot][:, c, j * 128:(j + 1) * 128], x1b[:, c, ts_], start=(c == 0), stop=(c == 7)),
                        rd=[WG[slot], X1B], wr=[PSG[q]])
                for c in range(8):
                    P.op("pe", lambda e, c=c, j=j, q=q, ts_=ts_, slot=slot: e.matmul(
                        psU[q][:, :], wus[slot][:, c, j * 128:(j + 1) * 128], x1b[:, c, ts_], start=(c == 0), stop=(c == 7)),
                        rd=[WU[slot], X1B], wr=[PSU[q]])
                P.op("act", lambda e, q=q: e.activation(out=sg[q][:, :], in_=psG[q][:, :], func=AF.Silu), rd=[PSG[q]], wr=[SG[q]])
                if debug == 3 and it == 0 and j == 0:
                    OB = Buf()
                    P.dma("sp", x2v[:, 0, 0:512], sg[q][:, :], rd=[SG[q]], wr=[OB])
                    P.fence("sp", [OB])
                    P.emit()
                    P.close()
                    return nc, P
                if E:
                    P.op("pool", lambda e, q=q, ts_=ts_, gb=gateB[ex % 2]: e.tensor_tensor(
                        out=sg[q][:, :], in0=sg[q][:, :], in1=gb[:, ts_], op=ALU.mult), rd=[SG[q], GB[ex % 2]], wr=[SG[q]])
                P.op("dve", lambda e, q=q, j=j, hb=hb: e.tensor_tensor(out=hb[:, j, :], in0=psU[q][:, :], in1=sg[q][:, :], op=ALU.mult),
                     rd=[PSU[q], SG[q]], wr=[HB])
            for c in range(8):
                q = c % 2
                for j in range(ng):
                    P.op("pe", lambda e, c=c, j=j, q=q, hb=hb, slot=slot, ng=ng: e.matmul(
                        psD[q][:, :], wds[slot][:, j, c * 128:(c + 1) * 128], hb[:, j, :], start=(j == 0), stop=(j == ng - 1)),
                        rd=[WD[slot], HB], wr=[PSD[q]])
                if gi == 0 or debug == 8:
                    P.op("act", lambda e, c=c, q=q, ts_=ts_: e.activation(out=acc[:, c, ts_], in_=psD[q][:, :], func=AF.Copy),
                         rd=[PSD[q]], wr=[ACC[t]])
                else:
                    P.op("dve", lambda e, c=c, q=q, ts_=ts_: e.tensor_tensor(out=acc[:, c, ts_], in0=acc[:, c, ts_], in1=psD[q][:, :], op=ALU.add),
                         rd=[PSD[q], ACC[t]], wr=[ACC[t]])
            if debug in (4, 5, 7) and it == 0:
                OB = Buf()
                P.dma("sp", x2v[:, :, 0:512], acc[:, :, 0:512], rd=[ACC[0]], wr=[OB])
                P.fence("sp", [OB])
                P.emit()
                P.close()
                return nc, P
            it += 1

    tmp = P.sb("tmp", [128, 3, 512], F32)
    TMP = Buf()
    S0 = [WG[0], WU[0], WD[0]]
    S1 = [WG[1], WU[1], WD[1]]
    outs = []
    for t in range(NT):
        ts_ = slice(t * 512, (t + 1) * 512)
        if debug in (1, 8, 9, 10, 11):
            OB = Buf()
            P.dma("sp", x2v[:, :, ts_], (acc2 if debug in (9, 10, 11) else acc)[:, :, ts_], rd=[ACC[t]], wr=[OB])
            outs.append(OB)
            continue
        P.dma("sp", ovl[1], x1v[:, :, ts_], wr=S1)
        P.op("dve", lambda e, ts_=ts_: e.scalar_tensor_tensor(out=acc[:, :, ts_], in0=ovl[1], scalar=float(alpha),
                                                               in1=acc[:, :, ts_], op0=ALU.mult, op1=ALU.add),
             rd=S1 + [ACC[t]], wr=[ACC[t]])
        if debug == 2:
            OB = Buf()
            P.dma("sp", x2v[:, :, ts_], acc[:, :, ts_], rd=[ACC[t]], wr=[OB])
            outs.append(OB)
            continue
        layer_norm(P, acc[:, :, ts_], ACC[t], ovl[0], S0, lnp_s[:, 0, :], lnp_s[:, 1, :], ovl[1], S1,
                   ones_m[:, :], psG[0][:, :], PSG[0], psU[0][:, :], PSU[0], tmp[:, :, :], TMP, extra=[LNP, ONES])
        OB = Buf()
        P.dma("sp", x2v[:, :, ts_], ovl[1], rd=S1, wr=[OB])
        outs.append(OB)
    P.fence("sp", outs)
    P.emit()
    P.close()
    return nc, P


RMS_EPS = 1e-6
N_IN = 7696


def build_p4a(Tc, alpha):
    nc = bass.Bass("TRN2", target_bir_lowering=False)
    P = Prog(nc)
    NT = Tc // 512
    oaT = P.dram("oaT", [1024, Tc], F32, "ExternalInput").rearrange("(c p) t -> p c t", p=128)
    obT = P.dram("obT", [512, Tc], F32, "ExternalInput").rearrange("(c p) t -> p c t", p=128)
    gT = P.dram("gT", [2048, Tc], F32, "ExternalInput").rearrange("(c p) t -> p c t", p=128)
    xT = P.dram("xT", [1024, Tc], F32, "ExternalInput").rearrange("(c p) t -> p c t", p=128)
    wa = P.dram("wa", [1024, 1024], F32, "ExternalInput").rearrange("(c p) n -> p c n", p=128)
    wb = P.dram("wb", [512, 1024], F32, "ExternalInput").rearrange("(c p) n -> p c n", p=128)
    wo = P.dram("wo", [1024, 1024], F32, "ExternalInput").rearrange("(c p) n -> p c n", p=128)
    lnp = P.dram("lnp", [128, 2, 8], F32, "ExternalInput")
    x1T = P.dram("x1T", [1024, Tc], F32, "ExternalOutput").rearrange("(c p) t -> p c t", p=128)

    was = P.sb("was", [128, 8, 1024], BF16)
    wbs = P.sb("wbs", [128, 4, 1024], BF16)
    wos = P.sb("wos", [128, 8, 1024], BF16)
    WA, WB, WO, LNP, ONES = Buf(), Buf(), Buf(), Buf(), Buf()
    lnp_s = P.sb("lnp_s", [128, 2, 8], F32)
    ones_m = P.sb("ones_m", [128, 128], F32)
    oa = P.sb("oa", [128, 8, 512], BF16)
    ob = P.sb("ob", [128, 4, 512], BF16)
    ga = P.sb("ga", [128, 8, 512], F32)
    gb = P.sb("gb", [128, 8, 512], F32)
    xs = P.sb("xs", [128, 8, 512], F32)
    OA, OB_, GA, GB, XS = Buf(), Buf(), Buf(), Buf(), Buf()
    u = P.sb("u", [128, 8, 512], BF16)
    U = Buf()
    t1 = [P.sb("t1_%d" % i, [128, 512], F32) for i in range(2)]
    t2 = [P.sb("t2_%d" % i, [128, 512], F32) for i in range(2)]
    T1, T2 = [Buf(), Buf()], [Buf(), Buf()]
    r32 = P.sb("r32", [128, 8, 512], F32)
    R32 = Buf()
    scr = P.sb("scr", [128, 8, 512], F32)
    SCR = Buf()
    o32 = P.sb("o32", [128, 8, 512], F32)
    O32 = Buf()
    tmp = P.sb("tmp", [128, 3, 512], F32)
    TMP = Buf()
    psA = [P.ps("psA%d" % i, [128, 512]) for i in range(2)]
    psB = [P.ps("psB%d" % i, [128, 512]) for i in range(2)]
    psM = [P.ps("psM%d" % i, [128, 512]) for i in range(2)]
    PSA, PSB, PSM = [Buf(), Buf()], [Buf(), Buf()], [Buf(), Buf()]

    P.dma("pool", was[:, :, :], wa, wr=[WA])
    P.dma("pool", wbs[:, :, :], wb, wr=[WB])
    P.dma("pool", wos[:, :, :], wo, wr=[WO])
    P.dma("sp", lnp_s[:, :, :], lnp, wr=[LNP])
    P.op("dve", lambda e: e.memset(ones_m[:, :], 1.0 / 1024.0), wr=[ONES])
    outs = []
    for t in range(NT):
        ts_ = slice(t * 512, (t + 1) * 512)
        P.dma("pool", oa[:, :, :], oaT[:, :, ts_], wr=[OA])
        P.dma("pool", ob[:, :, :], obT[:, :, ts_], wr=[OB_])
        P.dma("sp", ga[:, :, :], gT[:, 0:8, ts_], wr=[GA])
        P.dma("sp", gb[:, :, :], gT[:, 8:16, ts_], wr=[GB])
        P.dma("sp", xs[:, :, :], xT[:, :, ts_], wr=[XS])
        for c in range(8):
            q = c % 2
            cs = slice(c * 128, (c + 1) * 128)
            for k in range(8):
                P.op("pe", lambda e, k=k, q=q, cs=cs: e.matmul(psA[q][:, :], was[:, k, cs], oa[:, k, :], start=(k == 0), stop=(k == 7)),
                     rd=[WA, OA], wr=[PSA[q]])
            for k in range(4):
                P.op("pe", lambda e, k=k, q=q, cs=cs: e.matmul(psB[q][:, :], wbs[:, k, cs], ob[:, k, :], start=(k == 0), stop=(k == 3)),
                     rd=[WB, OB_], wr=[PSB[q]])
            P.op("dve", lambda e, c=c, q=q: e.tensor_tensor(out=t1[q][:, :], in0=psA[q][:, :], in1=ga[:, c, :], op=ALU.mult),
                 rd=[PSA[q], GA], wr=[T1[q]])
            P.op("dve", lambda e, c=c, q=q: e.tensor_tensor(out=t2[q][:, :], in0=psB[q][:, :], in1=gb[:, c, :], op=ALU.mult),
                 rd=[PSB[q], GB], wr=[T2[q]])
            P.op("pool", lambda e, c=c, q=q: e.tensor_tensor(out=u[:, c, :], in0=t1[q][:, :], in1=t2[q][:, :], op=ALU.add),
                 rd=[T1[q], T2[q]], wr=[U])
        for c in range(8):
            q = c % 2
            cs = slice(c * 128, (c + 1) * 128)
            for k in range(8):
                P.op("pe", lambda e, k=k, q=q, cs=cs: e.matmul(psM[q][:, :], wos[:, k, cs], u[:, k, :], start=(k == 0), stop=(k == 7)),
                     rd=[WO, U], wr=[PSM[q]])
            P.op("dve", lambda e, c=c, q=q: e.scalar_tensor_tensor(out=r32[:, c, :], in0=xs[:, c, :], scalar=float(alpha), in1=psM[q][:, :],
                                                                    op0=ALU.mult, op1=ALU.add), rd=[XS, PSM[q]], wr=[R32])
        layer_norm(P, r32[:, :, :], R32, scr[:, :, :], SCR, lnp_s[:, 0, :], lnp_s[:, 1, :], o32[:, :, :], O32,
                   ones_m[:, :], psA[0][:, :], PSA[0], psB[0][:, :], PSB[0], tmp[:, :, :], TMP, extra=[LNP, ONES])
        OBUF = Buf()
        P.dma("sp", x1T[:, :, ts_], o32[:, :, :], rd=[O32], wr=[OBUF])
        outs.append(OBUF)
    P.fence("sp", outs)
    P.emit()
    P.close()
    return nc, P


def p1_tiles():
    tl = []
    for i in range(8):
        tl.append((i * 128, 128, "q", "qkvT", i * 128))
    for i in range(8, 16):
        tl.append((i * 128, 128, "k", "qkvT", i * 128))
    for i in range(16, 24):
        tl.append((i * 128, 128, "v", "qkvT", i * 128))
    for i in range(8):
        tl.append((3072 + i * 128, 128, "z", "zT", i * 128))
    tl.append((4096, 8, "a", "gbT", 0))
    tl.append((4104, 8, "b", "gbT", 8))
    for i in range(12):
        tl.append((4112 + i * 128, 128, "mbq" if i < 4 else "mb", "mbT", i * 128))
    for i in range(16):
        tl.append((5648 + i * 128, 128, "gate", "gatesT", i * 128))
    return tl


def build_p1(Tc):
    nc = bass.Bass("TRN2", target_bir_lowering=False)
    P = Prog(nc)
    NT = Tc // 512
    TH = Tc + 3
    xTh = P.dram("xTh", [1024, TH], F32, "ExternalInput").rearrange("(c p) t -> p c t", p=128)
    w = P.dram("w", [1024, N_IN], F32, "ExternalInput").rearrange("(c p) n -> p c n", p=128)
    cw = P.dram("cw", [128, 24, 4], F32, "ExternalInput")
    ab = P.dram("ab", [8, 2], F32, "ExternalInput")
    outs_d = {
        "qkvT": P.dram("qkvT", [3072, Tc], F32, "ExternalOutput"),
        "zT": P.dram("zT", [1024, Tc], F32, "ExternalOutput"),
        "gbT": P.dram("gbT", [16, Tc], F32, "ExternalOutput"),
        "mbT": P.dram("mbT", [1536, Tc], F32, "ExternalOutput"),
        "gatesT": P.dram("gatesT", [2048, Tc], F32, "ExternalOutput"),
    }
    xb = P.sb("xb", [128, 8, TH], BF16)
    XB = Buf()
    cws = P.sb("cws", [128, 24, 4], F32)
    CW = Buf()
    abs_ = P.sb("abs", [8, 2], F32)
    AB = Buf()
    negA = P.sb("negA", [8, 1], F32)
    NEGA = Buf()
    ones_b = P.sb("ones_b", [128, 128], BF16)
    ONES = Buf()
    wt = [P.sb("wt%d" % i, [128, 8, 128], BF16) for i in range(2)]
    WT = [Buf(), Buf()]
    row = [P.sb("row%d" % i, [128, TH], F32) for i in range(2)]
    ROW = [Buf(), Buf()]
    s_ = [P.sb("s%d" % i, [128, Tc], F32) for i in range(2)]
    S_ = [Buf(), Buf()]
    sq = P.sb("sq", [128, Tc], BF16)
    SQ = Buf()
    rinv = [P.sb("rinv%d" % i, [128, 512], F32) for i in range(2)]
    RINV = [Buf(), Buf()]
    sm = [P.sb("sm%d" % i, [8, Tc], F32) for i in range(3)]
    SM = [Buf(), Buf(), Buf()]
    psP = P.ps("psP", [128, NT * 512])
    PSP = [Buf() for _ in range(NT)]
    psH = P.ps("psH", [128, 512])
    PSH = Buf()
    psS = [P.ps("psS%d" % i, [128, 512]) for i in range(2)]
    PSS = [Buf(), Buf()]

    P.dma("pool", xb[:, :, :], xTh, wr=[XB])
    P.dma("sp", cws[:, :, :], cw, wr=[CW])
    P.dma("sp", abs_[:, :], ab, wr=[AB])
    P.op("dve", lambda e: e.memset(ones_b[:, :], 1.0), wr=[ONES])
    P.op("act", lambda e: e.activation(out=negA[:, :], in_=abs_[:, 0:1], func=AF.Exp), rd=[AB], wr=[NEGA])
    P.op("dve", lambda e: e.tensor_scalar(out=negA[:, :], in0=negA[:, :], scalar1=-1.0, scalar2=None, op0=ALU.mult), rd=[NEGA], wr=[NEGA])

    tiles = p1_tiles()
    outs = []

    def load_w(i):
        c0, M, kind, on, r0 = tiles[i]
        P.dma("pool", wt[i % 2][:, :, 0:M], w[:, :, c0:c0 + M], wr=[WT[i % 2]])

    load_w(0)
    nconv = 0
    for i, (c0, M, kind, on, r0) in enumerate(tiles):
        if i + 1 < len(tiles):
            load_w(i + 1)
        sl = i % 2
        conv = kind in ("q", "k", "v")
        for t in range(NT):
            for k in range(8):
                P.op("pe", lambda e, k=k, t=t, sl=sl, M=M: e.matmul(psP[0:M, t * 512:(t + 1) * 512], wt[sl][:, k, 0:M],
                                                                   xb[:, k, 3 + t * 512:3 + (t + 1) * 512], start=(k == 0), stop=(k == 7)),
                     rd=[WT[sl], XB], wr=[PSP[t]])
        dst = outs_d[on][r0:r0 + M, :]
        OBUF = Buf()
        if conv:
            ct = c0 // 128
            b2 = nconv % 2
            nconv += 1
            for k in range(8):
                P.op("pe", lambda e, k=k, sl=sl: e.matmul(psH[:, 0:3], wt[sl][:, k, :], xb[:, k, 0:3], start=(k == 0), stop=(k == 7)),
                     rd=[WT[sl], XB], wr=[PSH])
            P.op("act", lambda e, b2=b2: e.activation(out=row[b2][:, 0:3], in_=psH[:, 0:3], func=AF.Copy), rd=[PSH], wr=[ROW[b2]])
            P.op("act", lambda e, b2=b2: e.activation(out=row[b2][:, 3:TH], in_=psP[:, :], func=AF.Copy), rd=PSP, wr=[ROW[b2]])
            sb_ = s_[b2]
            P.op("dve", lambda e, b2=b2, ct=ct, sb_=sb_: e.tensor_scalar(out=sb_[:, :], in0=row[b2][:, 3:3 + Tc], scalar1=cws[:, ct, 3:4], scalar2=None,
                                                                        op0=ALU.mult), rd=[ROW[b2], CW], wr=[S_[b2]])
            for wi in range(3):
                P.op("dve", lambda e, b2=b2, ct=ct, sb_=sb_, wi=wi: e.scalar_tensor_tensor(
                    out=sb_[:, :], in0=row[b2][:, wi:wi + Tc], scalar=cws[:, ct, wi:wi + 1], in1=sb_[:, :], op0=ALU.mult, op1=ALU.add),
                    rd=[ROW[b2], CW, S_[b2]], wr=[S_[b2]])
            P.op("act", lambda e, sb_=sb_: e.activation(out=sb_[:, :], in_=sb_[:, :], func=AF.Silu), rd=[S_[b2]], wr=[S_[b2]])
            if kind in ("q", "k"):
                scale = (128.0 ** -0.5) if kind == "q" else 1.0
                P.op("dve", lambda e, sb_=sb_: e.tensor_tensor(out=sq[:, :], in0=sb_[:, :], in1=sb_[:, :], op=ALU.mult), rd=[S_[b2]], wr=[SQ])
                for t in range(NT):
                    q = t % 2
                    ts_ = slice(t * 512, (t + 1) * 512)
                    P.op("pe", lambda e, q=q, ts_=ts_: e.matmul(psS[q][:, :], ones_b[:, :], sq[:, ts_], start=True, stop=True),
                         rd=[ONES, SQ], wr=[PSS[q]])
                    rsqrt_eps(P, rinv[q][:, :], psS[q][:, :], RMS_EPS, [PSS[q]], [RINV[q]])
                    P.op("dve", lambda e, q=q, ts_=ts_, sb_=sb_, scale=scale: e.scalar_tensor_tensor(
                        out=sb_[:, ts_], in0=sb_[:, ts_], scalar=float(scale), in1=rinv[q][:, :], op0=ALU.mult, op1=ALU.mult),
                        rd=[S_[b2], RINV[q]], wr=[S_[b2]])
            P.dma("sp", dst, sb_[:, :], rd=[S_[b2]], wr=[OBUF])
        elif kind in ("z", "gate", "mb", "mbq"):
            b2 = nconv % 2
            nconv += 1
            sb_ = s_[b2]
            func = {"z": AF.Silu, "gate": AF.Sigmoid, "mb": AF.Copy, "mbq": AF.Copy}[kind]
            scl = 0.125 if kind == "mbq" else 1.0
            P.op("act", lambda e, sb_=sb_, func=func, scl=scl: e.activation(out=sb_[:, :], in_=psP[:, :], func=func, scale=float(scl)),
                 rd=PSP, wr=[S_[b2]])
            P.dma("sp", dst, sb_[:, :], rd=[S_[b2]], wr=[OBUF])
        elif kind == "a":
            ta, t2_, t3_ = sm[0], sm[1], sm[2]
            P.op("act", lambda e: e.activation(out=ta[:, :], in_=psP[0:8, :], func=AF.Identity, bias=abs_[:, 1:2], scale=1.0),
                 rd=PSP + [AB], wr=[SM[0]])
            P.op("act", lambda e: e.activation(out=t2_[:, :], in_=ta[:, :], func=AF.Abs), rd=[SM[0]], wr=[SM[1]])
            P.op("act", lambda e: e.activation(out=t2_[:, :], in_=t2_[:, :], func=AF.Exp, scale=-1.0), rd=[SM[1]], wr=[SM[1]])
            P.op("act", lambda e: e.activation(out=t2_[:, :], in_=t2_[:, :], func=AF.Ln, bias=1.0, scale=1.0), rd=[SM[1]], wr=[SM[1]])
            P.op("dve", lambda e: e.scalar_tensor_tensor(out=t3_[:, :], in0=ta[:, :], scalar=0.0, in1=t2_[:, :], op0=ALU.max, op1=ALU.add),
                 rd=[SM[0], SM[1]], wr=[SM[2]])
            P.op("dve", lambda e: e.tensor_scalar(out=t3_[:, :], in0=t3_[:, :], scalar1=negA[:, 0:1], scalar2=None, op0=ALU.mult),
                 rd=[SM[2], NEGA], wr=[SM[2]])
            P.dma("sp", dst, t3_[:, :], rd=[SM[2]], wr=[OBUF])
        elif kind == "b":
            P.op("act", lambda e: e.activation(out=sm[0][:, :], in_=psP[0:8, :], func=AF.Sigmoid), rd=PSP, wr=[SM[0]])
            P.dma("sp", dst, sm[0][:, :], rd=[SM[0]], wr=[OBUF])
        outs.append(OBUF)
    P.fence("sp", outs)
    P.emit()
    P.close()
    return nc, P


RMS_EPS = 1e-6


def build_p2(S, HL=4, stop=99):
    nc = bass.Bass("TRN2", target_bir_lowering=False)
    P = Prog(nc)
    NCH = S // 128
    W = HL * 128
    kT = P.dram("kT", [HL, 128, S], F32, "ExternalInput")
    qT = P.dram("qT", [HL, 128, S], F32, "ExternalInput")
    ktok = P.dram("ktok", [S, HL, 128], F32, "ExternalInput")
    vtok = P.dram("vtok", [S, HL, 128], F32, "ExternalInput")
    ztok = P.dram("ztok", [S, HL, 128], F32, "ExternalInput")
    gtok = P.dram("gtok", [S, HL], F32, "ExternalInput")
    btok = P.dram("btok", [S, HL], F32, "ExternalInput")
    nw = P.dram("nw", [1, 128], F32, "ExternalInput")
    cst = P.dram("cst", [128, 5, 128], F32, "ExternalInput")
    oa = P.dram("oa", [S, HL, 128], F32, "ExternalOutput")

    cs = P.sb("cs", [128, 5, 128], F32)
    CS = Buf()
    ident, uincl, lstrict, uinc01, ustrict = cs[:, 0, :], cs[:, 1, :], cs[:, 2, :], cs[:, 3, :], cs[:, 4, :]
    ones = P.sb("ones", [128, 128], F32)
    ONES = Buf()
    nws = P.sb("nws", [128, 128], F32)
    NW = Buf()
    St = P.sb("St", [128, HL, 128], F32)
    ST = Buf()

    def t3(name, n=2):
        return [P.sb("%s%d" % (name, i), [128, HL, 128], F32) for i in range(n)], [Buf() for _ in range(n)]

    def t2(name, n=2):
        return [P.sb("%s%d" % (name, i), [128, HL], F32) for i in range(n)], [Buf() for _ in range(n)]

    kTs, KT = t3("kTs")
    qTs, QT = t3("qTs")
    kts, KTK = t3("kts")
    vts, VT = t3("vts")
    zts, ZT = t3("zts")
    gs, GS = t2("gs")
    bs, BS = t2("bs")
    (dec,), (DEC,) = t2("dec", 1)
    (ed,), (ED,) = t2("ed", 1)
    (be,), (BE,) = t2("be", 1)
    (nb,), (NB,) = t2("nb", 1)
    (gl,), (GL,) = t2("gl", 1)
    (ss,), (SS,) = t2("ss", 1)
    (rhsD,), (RHSD,) = t3("rhsD", 1)
    (E_,), (EE,) = t3("E", 1)
    (Qa, Qb), (QA, QB) = t3("Qm")
    (Pa, Pb), (PA, PB) = t3("Pm")
    (X_,), (XX,) = t3("X", 1)
    (IT,), (ITB,) = t3("IT", 1)
    (vb,), (VB,) = t3("vb", 1)
    (kd,), (KD,) = t3("kd", 1)
    (R_,), (RR,) = t3("R", 1)
    (vn,), (VN,) = t3("vn", 1)
    (tmp,), (TMP,) = t3("tmp", 1)
    o_, OO = t3("o")
    ps = [P.ps("ps%d" % i, [128, 512]) for i in range(8)]
    PS = [Buf() for _ in range(8)]

    def v3(bank):
        return ps[bank][:, 0:W].rearrange("p (h f) -> p h f", h=HL)

    P.dma("sp", cs[:, :, :], cst, wr=[CS])
    P.dma("sp", nws[:, :], nw.partition_broadcast(128), wr=[NW])
    P.op("dve", lambda e: e.memset(ones[:, :], 1.0), wr=[ONES])
    P.op("dve", lambda e: e.memset(St[:, :, :], 0.0), wr=[ST])
    P.op("dve", lambda e: e.tensor_scalar(out=nws[:, :], in0=nws[:, :], scalar1=float(128.0 ** 0.5), scalar2=None, op0=ALU.mult), rd=[NW], wr=[NW])

    def loads(c):
        b = c % 2
        tsl = slice(c * 128, (c + 1) * 128)
        P.dma("sp", kTs[b][:, :, :], kT[:, :, tsl].rearrange("h d t -> d h t"), wr=[KT[b]])
        P.dma("sp", qTs[b][:, :, :], qT[:, :, tsl].rearrange("h d t -> d h t"), wr=[QT[b]])
        P.dma("sp", kts[b][:, :, :], ktok[tsl, :, :], wr=[KTK[b]])
        P.dma("sp", vts[b][:, :, :], vtok[tsl, :, :], wr=[VT[b]])
        P.dma("sp", zts[b][:, :, :], ztok[tsl, :, :], wr=[ZT[b]])
        P.dma("sp", gs[b][:, :], gtok[tsl, :], wr=[GS[b]])
        P.dma("sp", bs[b][:, :], btok[tsl, :], wr=[BS[b]])

    def mm4(bank, lhs, LB, rhs, RB):
        for h in range(HL):
            P.op("pe", lambda e, h=h: e.matmul(ps[bank][:, h * 128:(h + 1) * 128], lhs[:, h, :], rhs[:, h, :], start=True, stop=True),
                 rd=[LB, RB], wr=[PS[bank]])

    def tt(eng, out, OB, a, AB, b, BB, op):
        rd = [x for x in (AB, BB) if x is not None]
        P.op(eng, lambda e: e.tensor_tensor(out=out, in0=a, in1=b, op=op), rd=rd, wr=[OB])

    loads(0)
    outs = []
    for c in range(NCH):
        b = c % 2
        if c + 1 < NCH:
            loads(c + 1)
        k_T, q_T, k_t, v_t, z_t, g_, b_ = kTs[b], qTs[b], kts[b], vts[b], zts[b], gs[b], bs[b]
        P.op("pe", lambda e, g_=g_: e.matmul(ps[0][:, 0:HL], uincl, g_[:, :], start=True, stop=True), rd=[CS, GS[b]], wr=[PS[0]])
        P.op("dve", lambda e: e.tensor_copy(out=dec[:, :], in_=ps[0][:, 0:HL]), rd=[PS[0]], wr=[DEC])
        tt("dve", rhsD[:, :, :], RHSD, bcast_mid(ident, HL), CS, bcast_last(dec[:, :], 128), DEC, ALU.mult)
        P.op("pe", lambda e: e.matmul(ps[0][:, 0:W], ones[:, :], rhsD[:, :, :].rearrange("p h f -> p (h f)"), start=True, stop=True),
             rd=[ONES, RHSD], wr=[PS[0]])
        P.op("act", lambda e: e.activation(out=gl[:, :], in_=v3(0)[:, :, 127], func=AF.Exp), rd=[PS[0]], wr=[GL])
        tt("dve", E_[:, :, :], EE, v3(0), PS[0], bcast_last(dec[:, :], 128), DEC, ALU.subtract)
        P.op("act", lambda e: e.activation(out=E_[:, :, :], in_=E_[:, :, :], func=AF.Abs), rd=[EE], wr=[EE])
        P.op("act", lambda e: e.activation(out=E_[:, :, :], in_=E_[:, :, :], func=AF.Exp, scale=-1.0), rd=[EE], wr=[EE])
        P.op("act", lambda e: e.activation(out=ed[:, :], in_=dec[:, :], func=AF.Exp), rd=[DEC], wr=[ED])
        tt("dve", be[:, :], BE, b_[:, :], BS[b], ed[:, :], ED, ALU.mult)
        P.op("dve", lambda e, b_=b_: e.tensor_scalar(out=nb[:, :], in0=b_[:, :], scalar1=-1.0, scalar2=None, op0=ALU.mult), rd=[BS[b]], wr=[NB])
        if stop == 1:
            OBUF = Buf()
            P.dma("sp", oa[0:128, :, :], E_[:, :, :], rd=[EE], wr=[OBUF])
            P.fence("sp", [OBUF])
            P.emit()
            P.close()
            return nc, P
        mm4(1, k_T, KT[b], k_T, KT[b])
        tt("dve", Qa[:, :, :], QA, v3(1), PS[1], E_[:, :, :], EE, ALU.mult)
        if stop == 6:
            OBUF = Buf()
            P.dma("sp", oa[0:128, :, :], Qa[:, :, :], rd=[QA], wr=[OBUF])
            P.fence("sp", [OBUF])
            P.emit()
            P.close()
            return nc, P
        mm4(2, k_T, KT[b], q_T, QT[b])
        tt("dve", IT[:, :, :], ITB, v3(2), PS[2], E_[:, :, :], EE, ALU.mult)
        tt("dve", IT[:, :, :], ITB, IT[:, :, :], ITB, bcast_mid(uinc01, HL), CS, ALU.mult)
        if stop == 7:
            OBUF = Buf()
            P.dma("sp", oa[0:128, :, :], Qa[:, :, :], rd=[QA], wr=[OBUF])
            P.fence("sp", [OBUF])
            P.emit()
            P.close()
            return nc, P
        tt("dve", rhsD[:, :, :], RHSD, bcast_mid(ident, HL), CS, bcast_last(nb[:, :], 128), NB, ALU.mult)
        P.op("pe", lambda e: e.matmul(ps[3][:, 0:W], ones[:, :], rhsD[:, :, :].rearrange("p h f -> p (h f)"), start=True, stop=True),
             rd=[ONES, RHSD], wr=[PS[3]])
        tt("dve", Pa[:, :, :], PA, v3(3), PS[3], Qa[:, :, :], QA, ALU.mult)
        tt("dve", Pa[:, :, :], PA, Pa[:, :, :], PA, bcast_mid(ustrict, HL), CS, ALU.mult)
        tt("dve", Qa[:, :, :], QA, Qa[:, :, :], QA, bcast_last(nb[:, :], 128), NB, ALU.mult)
        tt("dve", Qa[:, :, :], QA, Qa[:, :, :], QA, bcast_mid(lstrict, HL), CS, ALU.mult)
        tt("dve", X_[:, :, :], XX, Pa[:, :, :], PA, bcast_mid(ident, HL), CS, ALU.add)
        if stop == 2:
            OBUF = Buf()
            P.dma("sp", oa[0:128, :, :], E_[:, :, :], rd=[EE], wr=[OBUF])
            P.fence("sp", [OBUF])
            P.emit()
            P.close()
            return nc, P
        tt("dve", vb[:, :, :], VB, v_t[:, :, :], VT[b], bcast_last(b_[:, :], 128), BS[b], ALU.mult)
        tt("dve", kd[:, :, :], KD, k_t[:, :, :], KTK[b], bcast_last(E_[:, :, 127], 128), EE, ALU.mult)
        if stop == 3:
            OBUF = Buf()
            P.dma("sp", oa[0:128, :, :], E_[:, :, :], rd=[EE], wr=[OBUF])
            P.fence("sp", [OBUF])
            P.emit()
            P.close()
            return nc, P
        Qc, QC, Qn, QN = Qa, QA, Qb, QB
        Pc, PC, Pn, PN = Pa, PA, Pb, PB
        for lev in range(6):
            mm4(4, Pc, PC, Qc, QC)
            P.op("act", lambda e, Qn=Qn: e.activation(out=Qn[:, :, :], in_=v3(4), func=AF.Copy), rd=[PS[4]], wr=[QN])
            if lev < 5:
                mm4(5, Qc, QC, Pc, PC)
                P.op("act", lambda e, Pn=Pn: e.activation(out=Pn[:, :, :], in_=v3(5), func=AF.Copy), rd=[PS[5]], wr=[PN])
            mm4(6, Qn, QN, X_, XX)
            tt("dve", X_[:, :, :], XX, X_[:, :, :], XX, v3(6), PS[6], ALU.add)
            Qc, QC, Qn, QN = Qn, QN, Qc, QC
            Pc, PC, Pn, PN = Pn, PN, Pc, PC
        if stop == 4:
            OBUF = Buf()
            P.dma("sp", oa[0:128, :, :], E_[:, :, :], rd=[EE], wr=[OBUF])
            P.fence("sp", [OBUF])
            P.emit()
            P.close()
            return nc, P
        mm4(7, k_T, KT[b], St, ST)
        tt("dve", tmp[:, :, :], TMP, v3(7), PS[7], bcast_last(be[:, :], 128), BE, ALU.mult)
        tt("dve", R_[:, :, :], RR, vb[:, :, :], VB, tmp[:, :, :], TMP, ALU.subtract)
        mm4(0, X_, XX, R_, RR)
        P.op("act", lambda e: e.activation(out=vn[:, :, :], in_=v3(0), func=AF.Copy), rd=[PS[0]], wr=[VN])
        mm4(1, q_T, QT[b], St, ST)
        mm4(2, IT, ITB, vn, VN)
        ob = o_[b]
        tt("dve", ob[:, :, :], OO[b], v3(1), PS[1], bcast_last(ed[:, :], 128), ED, ALU.mult)
        tt("dve", ob[:, :, :], OO[b], ob[:, :, :], OO[b], v3(2), PS[2], ALU.add)
        mm4(3, kd, KD, vn, VN)
        tt("dve", St[:, :, :], ST, St[:, :, :], ST, bcast_last(gl[:, :], 128), GL, ALU.mult)
        tt("dve", St[:, :, :], ST, St[:, :, :], ST, v3(3), PS[3], ALU.add)
        if stop == 5:
            OBUF = Buf()
            P.dma("sp", oa[0:128, :, :], E_[:, :, :], rd=[EE], wr=[OBUF])
            P.fence("sp", [OBUF])
            P.emit()
            P.close()
            return nc, P
        tt("dve", tmp[:, :, :], TMP, ob[:, :, :], OO[b], ob[:, :, :], OO[b], ALU.mult)
        P.op("dve", lambda e: e.tensor_reduce(out=ss[:, :], in_=tmp[:, :, :], axis=AX.X, op=ALU.add), rd=[TMP], wr=[SS])
        rsqrt_eps(P, ss[:, :], ss[:, :], 128.0 * RMS_EPS, [SS], [SS])
        tt("dve", ob[:, :, :], OO[b], ob[:, :, :], OO[b], bcast_last(ss[:, :], 128), SS, ALU.mult)
        tt("dve", ob[:, :, :], OO[b], ob[:, :, :], OO[b], bcast_mid(nws[:, :], HL), NW, ALU.mult)
        tt("pool", ob[:, :, :], OO[b], ob[:, :, :], OO[b], z_t[:, :, :], ZT[b], ALU.mult)
        OBUF = Buf()
        P.dma("sp", oa[c * 128:(c + 1) * 128, :, :], ob[:, :, :], rd=[OO[b]], wr=[OBUF])
        outs.append(OBUF)
    P.fence("sp", outs)
    P.emit()
    P.close()
    return nc, P


NEGB = -30000.0


def build_p3(S, HL=4):
    nc = bass.Bass("TRN2", target_bir_lowering=False)
    P = Prog(nc)
    NQ = S // 128
    NB = S // 256
    qT = P.dram("qT", [HL, 64, S], F32, "ExternalInput")
    kT = P.dram("kT", [HL, 64, S], F32, "ExternalInput")
    vtok = P.dram("vtok", [S, HL, 64], F32, "ExternalInput")
    rb = P.dram("rb", [HL, 32], F32, "ExternalInput")
    oh = P.dram("oh", [2, 31, 128, 384], F32, "ExternalInput")
    neg = P.dram("neg", [2, 128, 384], F32, "ExternalInput")
    pastm = P.dram("pastm", [128, NQ, NB], F32, "ExternalInput")
    ownm = P.dram("ownm", [128, NQ, NB], F32, "ExternalInput")
    ident_d = P.dram("ident", [128, 128], F32, "ExternalInput")
    ob = P.dram("ob", [S, HL, 64], F32, "ExternalOutput")

    idf = P.sb("idf", [128, 128], F32)
    idb = P.sb("idb", [128, 128], BF16)
    IDB = Buf()
    pm = P.sb("pm", [128, NQ, NB], F32)
    om = P.sb("om", [128, NQ, NB], F32)
    PM, OM = Buf(), Buf()
    qs = P.sb("qs", [64, S], BF16)
    ks = P.sb("ks", [64, S], BF16)
    vs = P.sb("vs", [128, S // 128, 64], BF16)
    QS, KS, VS = Buf(), Buf(), Buf()
    rbB = P.sb("rbB", [128, 32], F32)
    RB = Buf()
    T = [P.sb("T%d" % i, [128, 384], F32) for i in range(2)]
    TT = [Buf(), Buf()]
    ohs = [P.sb("ohs%d" % i, [128, 384], F32) for i in range(2)]
    OHS = [Buf(), Buf()]
    km = P.sb("km", [64, NB], F32)
    kmb = P.sb("kmb", [64, NB], BF16)
    KM, KMB = Buf(), Buf()
    sc = P.sb("sc", [128, NQ, NB], F32)
    s2 = P.sb("s2", [128, NQ, NB], F32)
    mb = P.sb("mb", [128, NQ, NB], F32)
    SC, S2, MB = Buf(), Buf(), Buf()
    mx = P.sb("mx", [128, NQ], F32)
    MX = Buf()
    L = P.sb("L", [128, S], F32)
    LB = Buf()
    Pm = P.sb("Pm", [128, S], BF16)
    PMB = Buf()
    st = P.sb("st", [128, 4], F32)
    STB = Buf()
    PT = [P.sb("PT%d" % i, [128, 128], BF16) for i in range(2)]
    PTB = [Buf(), Buf()]
    osb = [P.sb("osb%d" % i, [128, 64], F32) for i in range(2)]
    OSB = [Buf(), Buf()]
    psS = P.ps("psS", [128, 512])
    PSS = Buf()
    psL = [P.ps("psL%d" % i, [128, 512]) for i in range(2)]
    PSL = [Buf(), Buf()]
    psT = [P.ps("psT%d" % i, [128, 128], BF16) for i in range(2)]
    PST = [Buf(), Buf()]
    psO = P.ps("psO", [128, 64])
    PSO = Buf()

    P.dma("sp", idf[:, :], ident_d, wr=[IDB])
    P.op("dve", lambda e: e.tensor_copy(out=idb[:, :], in_=idf[:, :]), rd=[IDB], wr=[IDB])
    P.dma("sp", pm[:, :, :], pastm, wr=[PM])
    P.dma("sp", om[:, :, :], ownm, wr=[OM])
    outs = []
    noh = 0
    for h in range(HL):
        P.dma("pool", qs[:, :], qT[h], wr=[QS])
        P.dma("pool", ks[:, :], kT[h], wr=[KS])
        P.dma("pool", vs[:, :, :], vtok[:, h, :].rearrange("(t p) d -> p t d", p=128), wr=[VS])
        P.dma("sp", rbB[:, :], rb[h:h + 1, :].partition_broadcast(128), wr=[RB])
        P.op("dve", lambda e: e.tensor_scalar(out=rbB[:, 0:31], in0=rbB[:, 0:31], scalar1=rbB[:, 31:32], scalar2=None, op0=ALU.subtract),
             rd=[RB], wr=[RB])
        for pos in range(2):
            P.dma("sp", T[pos][:, :], neg[pos], wr=[TT[pos]])
            for b in range(31):
                sl = noh % 2
                noh += 1
                P.dma("sp", ohs[sl][:, :], oh[pos, b], wr=[OHS[sl]])
                P.op("dve", lambda e, pos=pos, b=b, sl=sl: e.scalar_tensor_tensor(out=T[pos][:, :], in0=ohs[sl][:, :], scalar=rbB[:, b:b + 1],
                                                                                 in1=T[pos][:, :], op0=ALU.mult, op1=ALU.add),
                     rd=[OHS[sl], RB, TT[pos]], wr=[TT[pos]])
        P.op("dve", lambda e: e.tensor_reduce(out=km[:, :], in_=ks[:, :].rearrange("p (n k) -> p n k", k=256), axis=AX.X, op=ALU.add),
             rd=[KS], wr=[KM])
        P.op("dve", lambda e: e.tensor_scalar(out=kmb[:, :], in0=km[:, :], scalar1=1.0 / 256.0, scalar2=None, op0=ALU.mult), rd=[KM], wr=[KMB])
        for qt in range(NQ):
            P.op("pe", lambda e, qt=qt: e.matmul(psS[:, qt * NB:(qt + 1) * NB], qs[:, qt * 128:(qt + 1) * 128], kmb[:, :], start=True, stop=True),
                 rd=[QS, KMB], wr=[PSS])
        psSv = psS[:, 0:NQ * NB].rearrange("p (q n) -> p q n", n=NB)
        P.op("dve", lambda e: e.tensor_tensor(out=sc[:, :, :], in0=psSv, in1=pm[:, :, :], op=ALU.add), rd=[PSS, PM], wr=[SC])
        P.op("dve", lambda e: e.tensor_copy(out=s2[:, :, :], in_=sc[:, :, :]), rd=[SC], wr=[S2])
        for r in range(3):
            P.op("dve", lambda e: e.tensor_reduce(out=mx[:, :], in_=s2[:, :, :], axis=AX.X, op=ALU.max), rd=[S2], wr=[MX])
            if r < 2:
                P.op("dve", lambda e: e.tensor_tensor(out=mb[:, :, :], in0=s2[:, :, :], in1=bcast_last(mx[:, :], NB), op=ALU.is_ge),
                     rd=[S2, MX], wr=[MB])
                P.op("dve", lambda e: e.scalar_tensor_tensor(out=s2[:, :, :], in0=mb[:, :, :], scalar=-1e30, in1=s2[:, :, :], op0=ALU.mult, op1=ALU.add),
                     rd=[MB, S2], wr=[S2])
        P.op("dve", lambda e: e.tensor_scalar(out=mx[:, :], in0=mx[:, :], scalar1=-1e29, scalar2=None, op0=ALU.max), rd=[MX], wr=[MX])
        P.op("dve", lambda e: e.tensor_tensor(out=mb[:, :, :], in0=sc[:, :, :], in1=bcast_last(mx[:, :], NB), op=ALU.is_ge), rd=[SC, MX], wr=[MB])
        P.op("dve", lambda e: e.tensor_tensor(out=mb[:, :, :], in0=mb[:, :, :], in1=om[:, :, :], op=ALU.max), rd=[MB, OM], wr=[MB])
        P.op("dve", lambda e: e.tensor_scalar(out=mb[:, :, :], in0=mb[:, :, :], scalar1=-1.0, scalar2=None, op0=ALU.add), rd=[MB], wr=[MB])
        P.op("dve", lambda e: e.tensor_scalar(out=mb[:, :, :], in0=mb[:, :, :], scalar1=-NEGB, scalar2=None, op0=ALU.mult), rd=[MB], wr=[MB])
        nt = 0
        for qt in range(NQ):
            obk = qt // 2
            pos = qt % 2
            nb_ = obk + 1
            nk = nb_ * 256
            qsl = slice(qt * 128, (qt + 1) * 128)
            for p in range((nk + 511) // 512):
                w = min(512, nk - p * 512)
                pb = p % 2
                P.op("pe", lambda e, p=p, w=w, pb=pb, qsl=qsl: e.matmul(psL[pb][:, 0:w], qs[:, qsl], ks[:, p * 512:p * 512 + w], start=True, stop=True),
                     rd=[QS, KS], wr=[PSL[pb]])
                P.op("act", lambda e, p=p, w=w, pb=pb: e.activation(out=L[:, p * 512:p * 512 + w], in_=psL[pb][:, 0:w], func=AF.Copy),
                     rd=[PSL[pb]], wr=[LB])
            if obk >= 1:
                P.op("dve", lambda e, obk=obk, pos=pos: e.tensor_tensor(out=L[:, obk * 256 - 128:obk * 256 + 256], in0=L[:, obk * 256 - 128:obk * 256 + 256],
                                                                       in1=T[pos][:, :], op=ALU.add), rd=[LB, TT[pos]], wr=[LB])
            else:
                P.op("dve", lambda e, pos=pos: e.tensor_tensor(out=L[:, 0:256], in0=L[:, 0:256], in1=T[pos][:, 128:384], op=ALU.add),
                     rd=[LB, TT[pos]], wr=[LB])
            P.op("dve", lambda e, nk=nk, nb_=nb_, qt=qt: e.tensor_tensor(out=L[:, 0:nk].rearrange("p (n k) -> p n k", k=256),
                                                                        in0=L[:, 0:nk].rearrange("p (n k) -> p n k", k=256),
                                                                        in1=bcast_last(mb[:, qt, 0:nb_], 256), op=ALU.add), rd=[LB, MB], wr=[LB])
            P.op("dve", lambda e, nk=nk: e.tensor_reduce(out=st[:, 0:1], in_=L[:, 0:nk], axis=AX.X, op=ALU.max), rd=[LB], wr=[STB])
            P.op("dve", lambda e: e.tensor_scalar(out=st[:, 1:2], in0=st[:, 0:1], scalar1=-1.0, scalar2=None, op0=ALU.mult), rd=[STB], wr=[STB])
            P.op("act", lambda e, nk=nk: e.activation(out=Pm[:, 0:nk], in_=L[:, 0:nk], func=AF.Exp, bias=st[:, 1:2], scale=1.0),
                 rd=[LB, STB], wr=[PMB])
            P.op("dve", lambda e, nk=nk: e.tensor_reduce(out=st[:, 2:3], in_=Pm[:, 0:nk], axis=AX.X, op=ALU.add), rd=[PMB], wr=[STB])
            P.op("dve", lambda e: e.reciprocal(out=st[:, 3:4], in_=st[:, 2:3]), rd=[STB], wr=[STB])
            nkt = nk // 128
            for kt in range(nkt):
                tb = nt % 2
                nt += 1
                P.op("pe", lambda e, kt=kt, tb=tb: e.transpose(psT[tb][:, :], Pm[:, kt * 128:(kt + 1) * 128], idb[:, :]),
                     rd=[PMB, IDB], wr=[PST[tb]])
                eng = "act" if kt % 2 == 0 else "dve"
                if eng == "act":
                    P.op("act", lambda e, tb=tb: e.activation(out=PT[tb][:, :], in_=psT[tb][:, :], func=AF.Copy), rd=[PST[tb]], wr=[PTB[tb]])
                else:
                    P.op("dve", lambda e, tb=tb: e.tensor_copy(out=PT[tb][:, :], in_=psT[tb][:, :]), rd=[PST[tb]], wr=[PTB[tb]])
                P.op("pe", lambda e, kt=kt, tb=tb, nkt=nkt: e.matmul(psO[:, :], PT[tb][:, :], vs[:, kt, :], start=(kt == 0), stop=(kt == nkt - 1)),
                     rd=[PTB[tb], VS], wr=[PSO])
            ob_ = qt % 2
            P.op("dve", lambda e, ob_=ob_: e.tensor_scalar(out=osb[ob_][:, :], in0=psO[:, :], scalar1=st[:, 3:4], scalar2=None, op0=ALU.mult),
                 rd=[PSO, STB], wr=[OSB[ob_]])
            OBUF = Buf()
            P.dma("sp", ob[qsl, h, :], osb[ob_][:, :], rd=[OSB[ob_]], wr=[OBUF])
            outs.append(OBUF)
    P.fence("sp", outs)
    P.emit()
    P.close()
    return nc, P

import math as _math

_CACHE = {}


def _prog(key, fn):
    if key not in _CACHE:
        _CACHE[key] = fn()[0]
    return _CACHE[key]


def _run(nc, in_maps):
    res = run_bass_kernel_spmd(nc, in_maps, core_ids=list(range(8)))
    return res.results


def _bucket(d):
    n = np.maximum(d, 0)
    nf = np.maximum(n, 1).astype(np.float32)
    large = 16 + (np.log(nf / 16) / _math.log(128 / 16) * 16).astype(np.int32)
    large = np.minimum(large, 31)
    return np.where(n < 16, n, large)


def _moba_consts(S):
    NQ, NBk = S // 128, S // 256
    qq = np.arange(128)[:, None]
    kw = np.arange(384)[None, :]
    oh = np.zeros((2, 31, 128, 384), np.float32)
    neg = np.zeros((2, 128, 384), np.float32)
    for pos in range(2):
        d = pos * 128 + qq + 128 - kw
        bk = _bucket(d)
        for b in range(31):
            oh[pos, b] = ((bk == b) & (d >= 0)).astype(np.float32)
        neg[pos] = np.where(d < 0, -30000.0, 0.0)
    pastm = np.zeros((128, NQ, NBk), np.float32)
    ownm = np.zeros((128, NQ, NBk), np.float32)
    for qt in range(NQ):
        for j in range(NBk):
            pastm[:, qt, j] = 0.0 if j < qt // 2 else -1e30
            ownm[:, qt, j] = 1.0 if j == qt // 2 else 0.0
    return oh, neg, pastm, ownm


def kernel(x, w_in, conv_w, a_log, dt_bias, dn_norm_w, w_up_a, w_up_b, w_o, rel_bias,
           ln1_g, ln1_b, ln2_g, ln2_b, ffn_w_gate, ffn_w_up, ffn_w_down,
           router_w, router_b, exp_w_gate, exp_w_up, exp_w_down):
    f32 = np.float32
    x = np.asarray(x, f32)
    B, S, D = x.shape
    DEPTH = w_in.shape[0]
    alpha = (2 * DEPTH) ** 0.25
    Tc = S // 2
    C = np.ascontiguousarray
    idx = np.arange(128)
    ident = np.eye(128, dtype=f32)
    cst = C(np.stack([ident, (idx[:, None] <= idx[None, :]).astype(f32), (idx[:, None] > idx[None, :]).astype(f32),
                      (idx[None, :] >= idx[:, None]).astype(f32), (idx[None, :] > idx[:, None]).astype(f32)], axis=1))
    oh, neg, pastm, ownm = _moba_consts(S)
    nc1 = _prog(("p1", Tc), lambda: build_p1(Tc))
    nc2 = _prog(("p2", S), lambda: build_p2(S, 4))
    nc3 = _prog(("p3", S), lambda: build_p3(S, 4))
    nc4a = _prog(("p4a", Tc), lambda: build_p4a(Tc, alpha))
    cur = x
    for l in range(DEPTH):
        cw = C(np.asarray(conv_w[l], f32).T.reshape(24, 128, 4).transpose(1, 0, 2))
        ab = C(np.stack([np.asarray(a_log[l], f32), np.asarray(dt_bias[l], f32)], 1))
        wl = C(np.asarray(w_in[l], f32))
        ims = []
        for c in range(8):
            b, hf = c // 2, c % 2
            xh = np.zeros((Tc + 3, D), f32)
            if hf == 0:
                xh[3:] = cur[b, :Tc]
            else:
                xh[:] = cur[b, Tc - 3:]
            ims.append({"xTh": C(xh.T), "w": wl, "cw": cw, "ab": ab})
        r1 = _run(nc1, ims)
        cat = lambda name, b: np.concatenate([r1[2 * b][name], r1[2 * b + 1][name]], axis=1)
        ims2, ims3 = [], []
        for c in range(8):
            b, hg = c // 2, c % 2
            hs_ = slice(hg * 4, hg * 4 + 4)
            qkv = cat("qkvT", b)
            zT = cat("zT", b)
            gb = cat("gbT", b)
            mb_ = cat("mbT", b)
            qTa = qkv[0:1024].reshape(8, 128, S)[hs_]
            kTa = qkv[1024:2048].reshape(8, 128, S)[hs_]
            vTa = qkv[2048:3072].reshape(8, 128, S)[hs_]
            zTa = zT.reshape(8, 128, S)[hs_]
            ims2.append({"kT": C(kTa), "qT": C(qTa), "ktok": C(kTa.transpose(2, 0, 1)), "vtok": C(vTa.transpose(2, 0, 1)),
                         "ztok": C(zTa.transpose(2, 0, 1)), "gtok": C(gb[0:8][hs_].T), "btok": C(gb[8:16][hs_].T),
                         "nw": C(np.asarray(dn_norm_w[l], f32)[None, :]), "cst": cst})
            qb = mb_[0:512].reshape(8, 64, S)[hs_]
            kb = mb_[512:1024].reshape(8, 64, S)[hs_]
            vb_ = mb_[1024:1536].reshape(8, 64, S)[hs_]
            ims3.append({"qT": C(qb), "kT": C(kb), "vtok": C(vb_.transpose(2, 0, 1)), "rb": C(np.asarray(rel_bias, f32)[hs_]),
                         "oh": oh, "neg": neg, "pastm": pastm, "ownm": ownm, "ident": ident})
        r2 = _run(nc2, ims2)
        r3 = _run(nc3, ims3)
        lnp1 = C(np.stack([np.asarray(ln1_g[l], f32).reshape(8, 128).T, np.asarray(ln1_b[l], f32).reshape(8, 128).T], axis=1))
        lnp2 = C(np.stack([np.asarray(ln2_g[l], f32).reshape(8, 128).T, np.asarray(ln2_b[l], f32).reshape(8, 128).T], axis=1))
        ims4 = []
        for c in range(8):
            b, hf = c // 2, c % 2
            tsl = slice(hf * Tc, (hf + 1) * Tc)
            oa_ = np.concatenate([r2[2 * b]["oa"][tsl], r2[2 * b + 1]["oa"][tsl]], axis=1).reshape(Tc, 1024)
            ob_ = np.concatenate([r3[2 * b]["ob"][tsl], r3[2 * b + 1]["ob"][tsl]], axis=1).reshape(Tc, 512)
            ims4.append({"oaT": C(oa_.T), "obT": C(ob_.T), "gT": C(r1[c]["gatesT"]), "xT": C(cur[b, tsl].T),
                         "wa": C(np.asarray(w_up_a[l], f32)), "wb": C(np.asarray(w_up_b[l], f32)), "wo": C(np.asarray(w_o[l], f32)), "lnp": lnp1})
        r4 = _run(nc4a, ims4)
        i = l // 2
        if l % 2 == 0:
            F_ = ffn_w_gate.shape[-1]
            nc5 = _prog(("p4b", Tc, F_, 0), lambda: build_p4b(Tc, F_, 0, alpha))
            wts = {"wg": C(np.asarray(ffn_w_gate[i], f32)[None]), "wu": C(np.asarray(ffn_w_up[i], f32)[None]), "wd": C(np.asarray(ffn_w_down[i], f32)[None])}
        else:
            F_ = exp_w_gate.shape[-1]
            E_n = exp_w_gate.shape[1]
            nc5 = _prog(("p4b", Tc, F_, E_n), lambda: build_p4b(Tc, F_, E_n, alpha))
            wts = {"wg": C(np.asarray(exp_w_gate[i], f32)), "wu": C(np.asarray(exp_w_up[i], f32)), "wd": C(np.asarray(exp_w_down[i], f32)),
                   "rw": C(np.asarray(router_w[i], f32)), "rb": C(np.asarray(router_b[i], f32)[None, :]), "ident": ident}
        ims5 = [dict(x1T=C(r4[c]["x1T"]), lnp=lnp2, **wts) for c in range(8)]
        r5 = _run(nc5, ims5)
        nxt = np.empty_like(cur)
        for c in range(8):
            b, hf = c // 2, c % 2
            nxt[b, hf * Tc:(hf + 1) * Tc] = r5[c]["x2T"].T
        cur = nxt
    return cur
```
